# Optimizing a Trainium2 kernel written in Bass

```python
import jax, jax.numpy as jnp
from jax import lax
import numpy as np

D_MODEL = 2048
BATCH = 1
SEQ = 8192
DEPTH = 2

N_HEADS_A = D_MODEL // 256
HEAD_DIM_A = 128
WIDTH_A = N_HEADS_A * HEAD_DIM_A
MOBA_BLOCK = 256
MOBA_TOPK = 3
Q_CHUNK = 128
SGU_WIDTH = D_MODEL // 2
SGU_GROUPS = 8
SGU_GROUP_DIM = SGU_WIDTH // SGU_GROUPS
SGU_CHUNK = 128
N_MEM = 256
N_HEADS_M = 4
HEAD_DIM_M = D_MODEL // 8
WIDTH_M = N_HEADS_M * HEAD_DIM_M
D_FF = -(-8 * D_MODEL // (3 * 256)) * 256
N_BRANCH = 3
W_IN_COLS = 3 * WIDTH_A + 2 * SGU_WIDTH + WIDTH_M + N_BRANCH * D_MODEL
SPLITS = [WIDTH_A, 2 * WIDTH_A, 3 * WIDTH_A, 3 * WIDTH_A + SGU_WIDTH,
          3 * WIDTH_A + 2 * SGU_WIDTH, 3 * WIDTH_A + 2 * SGU_WIDTH + WIDTH_M]
NEG_INF = -1e30
EPS = 1e-6

kernel_name = 'hybrid_moba_gmlp_memxattn_block'


def rmsnorm(x, g):
    xf = x.astype(jnp.float32)
    y = xf * lax.rsqrt(jnp.mean(xf * xf, axis=-1, keepdims=True) + EPS)
    return (y * g.astype(jnp.float32)).astype(x.dtype)


def alibi_slopes(n_heads):
    return 2.0 ** (-8.0 * jnp.arange(1, n_heads + 1, dtype=jnp.float32) / n_heads)


def moba_attention(q, k, v):
    B, H, S, dh = q.shape
    nb = -(-S // MOBA_BLOCK)
    pad = nb * MOBA_BLOCK - S
    kb = jnp.pad(k, ((0, 0), (0, 0), (0, pad), (0, 0))).reshape(B, H, nb, MOBA_BLOCK, dh)
    vb = jnp.pad(v, ((0, 0), (0, 0), (0, pad), (0, 0))).reshape(B, H, nb, MOBA_BLOCK, dh)
    slopes = alibi_slopes(H)
    q = q * dh ** -0.5
    kmean = jnp.mean(kb.astype(jnp.float32), axis=3)
    gate = jnp.einsum('bhtd,bhnd->bhtn', q.astype(jnp.float32), kmean)
    q_block = jnp.arange(S) // MOBA_BLOCK
    past = jnp.arange(nb)[None, :] < q_block[:, None]
    gate = jnp.where(past, gate, NEG_INF)
    n_sel = min(MOBA_TOPK, nb)
    _, sel = lax.top_k(gate, n_sel)
    nq = S // Q_CHUNK
    qc = jnp.moveaxis(q.reshape(B, H, nq, Q_CHUNK, dh), 2, 0)
    selc = jnp.moveaxis(sel.reshape(B, H, nq, Q_CHUNK, n_sel), 2, 0)
    bi = jnp.arange(B)[:, None, None, None]
    hi = jnp.arange(H)[None, :, None, None]

    def one_chunk(args):
        qi, si, c = args
        t = c * Q_CHUNK + jnp.arange(Q_CHUNK)
        own = (c * Q_CHUNK) // MOBA_BLOCK
        k_sel = kb[bi, hi, si]
        v_sel = vb[bi, hi, si]
        s_pos = si[..., None] * MOBA_BLOCK + jnp.arange(MOBA_BLOCK)
        sc_sel = jnp.einsum('bhqd,bhqnkd->bhqnk', qi, k_sel).astype(jnp.float32)
        dist_sel = (t[None, None, :, None, None] - s_pos).astype(jnp.float32)
        sc_sel = sc_sel - slopes[None, :, None, None, None] * dist_sel
        valid = jnp.arange(n_sel) < own
        sc_sel = jnp.where(valid[:, None], sc_sel, NEG_INF)
        k_own = lax.dynamic_index_in_dim(kb, own, axis=2, keepdims=False)
        v_own = lax.dynamic_index_in_dim(vb, own, axis=2, keepdims=False)
        o_pos = own * MOBA_BLOCK + jnp.arange(MOBA_BLOCK)
        dist_own = (t[:, None] - o_pos[None, :]).astype(jnp.float32)
        sc_own = jnp.einsum('bhqd,bhkd->bhqk', qi, k_own).astype(jnp.float32)
        sc_own = jnp.where(dist_own >= 0, sc_own - slopes[None, :, None, None] * dist_own, NEG_INF)
        sc = jnp.concatenate([sc_sel.reshape(B, H, Q_CHUNK, n_sel * MOBA_BLOCK), sc_own], axis=-1)
        p = jax.nn.softmax(sc, axis=-1).astype(v.dtype)
        p_sel = p[..., :n_sel * MOBA_BLOCK].reshape(B, H, Q_CHUNK, n_sel, MOBA_BLOCK)
        p_own = p[..., n_sel * MOBA_BLOCK:]
        return (jnp.einsum('bhqnk,bhqnkd->bhqd', p_sel, v_sel)
                + jnp.einsum('bhqk,bhkd->bhqd', p_own, v_own))

    out = lax.map(one_chunk, (qc, selc, jnp.arange(nq)))
    return jnp.moveaxis(out, 0, 2).reshape(B, H, S, dh)


def spatial_gating(u, v, g, w_s, b_s):
    B, S, _ = u.shape
    u = jax.nn.gelu(u)
    v = rmsnorm(jax.nn.gelu(v), g)
    nc = S // SGU_CHUNK
    vc = v.reshape(B, nc, SGU_CHUNK, SGU_GROUPS, SGU_GROUP_DIM)
    w = w_s * jnp.tril(jnp.ones((SGU_CHUNK, SGU_CHUNK), dtype=w_s.dtype))
    mixed = jnp.einsum('gts,bnsgc->bntgc', w, vc) + b_s.T[None, None, :, :, None]
    return u * mixed.reshape(B, S, SGU_WIDTH)


def memory_attention(qm, km, vm):
    sc = jnp.einsum('bshd,bmhd->bhsm', qm, km).astype(jnp.float32) * HEAD_DIM_M ** -0.5
    p = jax.nn.softmax(sc, axis=-1).astype(vm.dtype)
    return jnp.einsum('bhsm,bmhd->bshd', p, vm)


def setup_inputs(seed: int = 0) -> dict:
    key = jax.random.key(seed)
    ks = jax.random.split(key, 20)
    f32 = jnp.float32

    def nrm(k, shape, fan_in):
        return jax.random.normal(k, shape, f32) * fan_in ** -0.5

    def gain(k, shape):
        return 1.0 + 0.01 * jax.random.normal(k, shape, f32)

    return {
        'x': jax.random.normal(ks[0], (BATCH, SEQ, D_MODEL), f32),
        'mem': jax.random.normal(ks[1], (BATCH, N_MEM, D_MODEL), f32),
        'g_mix': gain(ks[2], (DEPTH, D_MODEL)),
        'w_in': nrm(ks[3], (DEPTH, D_MODEL, W_IN_COLS), D_MODEL),
        'gq_a': gain(ks[4], (DEPTH, HEAD_DIM_A)),
        'gk_a': gain(ks[5], (DEPTH, HEAD_DIM_A)),
        'g_sgu': gain(ks[6], (DEPTH, SGU_WIDTH)),
        'w_sgu': nrm(ks[7], (DEPTH, SGU_GROUPS, SGU_CHUNK, SGU_CHUNK), SGU_CHUNK),
        'b_sgu': gain(ks[8], (DEPTH, SGU_GROUPS, SGU_CHUNK)),
        'gq_m': gain(ks[9], (DEPTH, HEAD_DIM_M)),
        'gk_m': gain(ks[10], (DEPTH, HEAD_DIM_M)),
        'g_mem': gain(ks[11], (DEPTH, D_MODEL)),
        'w_mem_kv': nrm(ks[12], (DEPTH, D_MODEL, 2 * WIDTH_M), D_MODEL),
        'w_branch': nrm(ks[13], (DEPTH, N_BRANCH, WIDTH_A, D_MODEL), WIDTH_A),
        'w_out': nrm(ks[14], (DEPTH, D_MODEL, D_MODEL), D_MODEL),
        'g_ffn': gain(ks[15], (DEPTH, D_MODEL)),
        'w_gate_up': nrm(ks[16], (DEPTH, D_MODEL, 2 * D_FF), D_MODEL),
        'w_down': nrm(ks[17], (DEPTH, D_FF, D_MODEL), D_FF),
    }


def reference(x, mem, g_mix, w_in, gq_a, gk_a, g_sgu, w_sgu, b_sgu, gq_m, gk_m, g_mem,
              w_mem_kv, w_branch, w_out, g_ffn, w_gate_up, w_down):
    B, S, _ = x.shape
    M = mem.shape[1]
    for l in range(DEPTH):
        h = rmsnorm(x, g_mix[l])
        z = h @ w_in[l]
        qa, ka, va, ub, vb, qm, gates = jnp.split(z, SPLITS, axis=-1)
        qa = rmsnorm(qa.reshape(B, S, N_HEADS_A, HEAD_DIM_A), gq_a[l]).transpose(0, 2, 1, 3)
        ka = rmsnorm(ka.reshape(B, S, N_HEADS_A, HEAD_DIM_A), gk_a[l]).transpose(0, 2, 1, 3)
        va = va.reshape(B, S, N_HEADS_A, HEAD_DIM_A).transpose(0, 2, 1, 3)
        ya = moba_attention(qa, ka, va).transpose(0, 2, 1, 3).reshape(B, S, WIDTH_A)
        yb = spatial_gating(ub, vb, g_sgu[l], w_sgu[l], b_sgu[l])
        kvm = rmsnorm(mem, g_mem[l]) @ w_mem_kv[l]
        km, vm = jnp.split(kvm, 2, axis=-1)
        km = rmsnorm(km.reshape(B, M, N_HEADS_M, HEAD_DIM_M), gk_m[l])
        vm = vm.reshape(B, M, N_HEADS_M, HEAD_DIM_M)
        qm = rmsnorm(qm.reshape(B, S, N_HEADS_M, HEAD_DIM_M), gq_m[l])
        ym = memory_attention(qm, km, vm).reshape(B, S, WIDTH_M)
        ga, gb, gm = jnp.split(jax.nn.sigmoid(gates), N_BRANCH, axis=-1)
        merged = (ga * (ya @ w_branch[l, 0]) + gb * (yb @ w_branch[l, 1])
                  + gm * (ym @ w_branch[l, 2]))
        x = x + merged @ w_out[l]
        hf = rmsnorm(x, g_ffn[l])
        gt, up = jnp.split(hf @ w_gate_up[l], 2, axis=-1)
        x = x + (jax.nn.silu(gt) * up) @ w_down[l]
    return x
```

```python
from contextlib import ExitStack

import numpy as np
import ml_dtypes

import concourse.bass as bass
import concourse.mybir as mybir
from concourse.bass_utils import run_bass_kernel_spmd

F32 = mybir.dt.float32
BF16 = mybir.dt.bfloat16
AF = mybir.ActivationFunctionType
ALU = mybir.AluOpType
AX = mybir.AxisListType

NCORE = 8
D = 2048
KC = 16
T = 1024
NH = 8
DFF = 5632
NFF = 44
EPS = 1e-6
WCOLS = 12288
SLOT_ELEMS = 4096
NW = 4
SAME_ENG_DIST = 8
STOP_AFTER = 99
KVW = 8192 + 8192 + 32

COMPUTE = ("pe", "act", "dve", "pool")


class Op:
    __slots__ = ("eng", "fn", "deps", "stream", "tick", "needs_inc", "idx", "dma", "waits", "inc_amt")

    def __init__(self, eng, fn, dma):
        self.eng = eng
        self.fn = fn
        self.dma = dma
        self.deps = []
        self.stream = None
        self.tick = 0
        self.needs_inc = False
        self.idx = 0
        self.waits = []
        self.inc_amt = 16


class Sched:
    def __init__(self, nc):
        self.nc = nc
        self.ops = {e: [] for e in ("pe", "act", "dve", "pool", "sp")}
        self.tok_w = {}
        self.tok_r = {}
        self.dma_count = {}
        self.sems = {}

    def op(self, eng, fn, reads=(), writes=(), dma_key=None, inc_amt=16):
        o = Op(eng, fn, dma_key is not None)
        o.idx = len(self.ops[eng])
        if o.dma:
            o.stream = ("dma", dma_key)
            self.dma_count[dma_key] = self.dma_count.get(dma_key, 0) + inc_amt
            o.tick = self.dma_count[dma_key]
            o.inc_amt = inc_amt
            o.needs_inc = True
        else:
            o.stream = eng
        deps = {}
        for t in reads:
            for p in self.tok_w.get(t, {}).values():
                deps[id(p)] = p
        for t in writes:
            for p in self.tok_w.get(t, {}).values():
                deps[id(p)] = p
            for p in self.tok_r.get(t, {}).values():
                deps[id(p)] = p
        o.deps = list(deps.values())
        for t in writes:
            self.tok_w[t] = {o.stream: o}
            self.tok_r[t] = {}
        for t in reads:
            if t in writes:
                continue
            self.tok_r.setdefault(t, {})[o.stream] = o
        self.ops[eng].append(o)
        return o

    def _finalize(self):
        for eng, lst in self.ops.items():
            known = {}
            for o in lst:
                need = {}
                for p in o.deps:
                    if p.dma:
                        key, val = p.stream, p.tick
                    else:
                        if p.eng == o.eng and not o.dma:
                            if o.eng == "pe" or (o.idx - p.idx) > SAME_ENG_DIST:
                                continue
                        key, val = p.eng, p.idx + 1
                    if known.get(key, 0) >= val:
                        continue
                    if key not in need or need[key][0] < val:
                        need[key] = (val, p)
                for k, (v, p) in need.items():
                    known[k] = v
                    if not p.dma:
                        p.needs_inc = True
                o.waits = [p for (v, p) in need.values()]
        self.ninc = {}
        for eng in COMPUTE:
            t = 0
            for o in self.ops[eng]:
                if o.dma:
                    continue
                if o.needs_inc:
                    t += 1
                    o.tick = t
                else:
                    o.tick = None
            self.ninc[eng] = t
        for eng, lst in self.ops.items():
            for o in lst:
                o.waits = [(p.stream, p.tick) for p in o.waits]

    def emit(self, stack):
        nc = self.nc
        self._finalize()
        print("sched: ops", {e: len(l) for e, l in self.ops.items()}, "incs", self.ninc, flush=True)
        for e_, n_ in self.ninc.items():
            assert n_ < 8000, ("too many semaphore increments on one engine (device limit ~8k)", e_, n_)
        KMAX = 3000
        nsem = [0]

        def newsem():
            nsem[0] += 1
            return stack.enter_context(nc.semaphore("sm%d" % nsem[0]))

        for eng in COMPUTE:
            nt = sum(1 for o in self.ops[eng] if (not o.dma) and o.needs_inc)
            for i in range((nt + KMAX - 1) // KMAX + 1):
                self.sems[(eng, i)] = newsem()
        for k in self.dma_count:
            assert self.dma_count[k] < KMAX, ("dma semaphore count too large", k, self.dma_count[k])
            self.sems[("dma", k)] = newsem()
        block = stack.enter_context(nc.Block())
        final_waits = [(self.sems[("dma", k)], c) for k, c in self.dma_count.items()]

        def semval(stream, tick):
            if isinstance(stream, str):
                return self.sems[(stream, (tick - 1) // KMAX)], (tick - 1) % KMAX + 1
            return self.sems[stream], tick

        def run(engname):
            def body(eng):
                for o in self.ops[engname]:
                    for s_, v in o.waits:
                        sem, val = semval(s_, v)
                        eng.wait_ge(sem, val)
                    ins = o.fn(eng)
                    if o.dma:
                        ins.then_inc(self.sems[o.stream], o.inc_amt)
                    elif o.needs_inc:
                        sem, _ = semval(o.eng, o.tick)
                        ins.then_inc(sem, 1)
                if engname == "sp":
                    for sem, v in final_waits:
                        eng.wait_ge(sem, v)
            return body

        block.tensor(run("pe"))
        block.scalar(run("act"))
        block.vector(run("dve"))
        block.gpsimd(run("pool"))
        block.sync(run("sp"))


class Rot:
    def __init__(self, items):
        self.items = list(items)
        self.i = 0

    def next(self):
        v = self.items[self.i % len(self.items)]
        self.i += 1
        return v


class Arena:
    def __init__(self, ap, nbytes):
        self.ap = ap
        self.nbytes = nbytes
        self.off = 0

    def alloc(self, shape, dt):
        n = int(np.prod(shape[1:]))
        nb = n * (4 if dt == F32 else 2)
        nb_al = (nb + 31) // 32 * 32
        assert self.off + nb_al <= self.nbytes, ("SBUF arena overflow", self.off, nb_al, self.nbytes)
        a = self.ap[:, self.off // 2:(self.off + nb) // 2]
        self.off += nb_al
        if dt == F32:
            a = a.bitcast(F32)
        if len(shape) == 3:
            a = a.rearrange("p (a b) -> p a b", a=shape[1])
        elif len(shape) == 4:
            a = a.rearrange("p (a b c) -> p a b c", a=shape[1], b=shape[2])
        return a

    def mark(self):
        return self.off

    def release(self, m):
        self.off = m


def build_program(n_layers):
    nc = bass.Bass("TRN2", target_bir_lowering=False)

    def din(name, shape, dt=F32):
        return nc.dram_tensor(name, list(shape), dt, kind="ExternalInput").ap()

    xT_in = din("xT", [D, T])
    memT_in = din("memT", [D, 256])
    ident_in = din("ident", [128, 128], BF16)
    rg_in = din("rg", [128, 512])
    etab_in = din("etab", [1, 8 * 32])
    gmask_in = din("gmask", [1, 8 * 32])
    tril_in = din("trilT", [128, 128], BF16)
    L = []
    for l in range(n_layers):
        s = "_%d" % l
        L.append(dict(
            w_in=din("w_in" + s, [D, WCOLS]),
            w_mem_kv=din("w_mem_kv" + s, [D, 2048]),
            w_branch=din("w_branch" + s, [3, 1024, D]),
            w_out=din("w_out" + s, [D, D]),
            w_gate_up=din("w_gate_up" + s, [D, 2 * DFF]),
            w_down=din("w_down" + s, [DFF, D]),
            gcols=din("gcols" + s, [128, 64]),
            g_sgu=din("g_sgu" + s, [1, 1024]),
            wsT=din("wsT" + s, [128, 8 * 128]),
            b_sgu=din("b_sgu" + s, [1, 8 * 128]),
        ))
    outT = nc.dram_tensor("outT", [D, T], F32, kind="ExternalOutput").ap()
    kvloc = [nc.dram_tensor("kvloc%d" % l, [128, KVW], BF16, kind="Internal").ap() for l in range(n_layers)]
    kvall = [nc.dram_tensor("kvall%d" % l, [NCORE * 128, KVW], BF16, kind="Internal").ap() for l in range(n_layers)]

    with ExitStack() as st:
        SB_BYTES = 211968
        arena_t = st.enter_context(nc.sbuf_tensor("arena", [128, SB_BYTES // 2], BF16))
        A = Arena(arena_t[:], SB_BYTES)
        psb = [st.enter_context(nc.psum_tensor("psb%d" % i, [128, 512], F32)) for i in range(8)]
        PS = [p[:] for p in psb]
        PSB = [p[:].bitcast(BF16) for p in psb]
        S = Sched(nc)

        def ACT(out, in_, func, reads, writes, **kw):
            S.op("act", lambda e: e.activation(out=out, in_=in_, func=func, **kw), reads, writes)

        def TT(out, in0, in1, op, reads, writes):
            S.op("dve", lambda e: e.tensor_tensor(out=out, in0=in0, in1=in1, op=op), reads, writes)

        def STT(out, in0, scalar, in1, op0, op1, reads, writes):
            S.op("dve", lambda e: e.scalar_tensor_tensor(out=out, in0=in0, scalar=scalar, in1=in1, op0=op0, op1=op1), reads, writes)

        def TS(out, in0, s1, s2, op0, op1, reads, writes):
            if op1 is None:
                S.op("dve", lambda e: e.tensor_scalar(out=out, in0=in0, scalar1=s1, scalar2=None, op0=op0), reads, writes)
            else:
                S.op("dve", lambda e: e.tensor_scalar(out=out, in0=in0, scalar1=s1, scalar2=s2, op0=op0, op1=op1), reads, writes)

        def RECIP(out, in_, reads, writes):
            S.op("dve", lambda e: e.reciprocal(out=out, in_=in_), reads, writes)

        def RSUM(out, in_, reads, writes):
            S.op("dve", lambda e: e.tensor_reduce(out=out, in_=in_, axis=AX.X, op=ALU.add), reads, writes)

        def CPY(eng, out, in_, reads, writes):
            if eng == "act":
                S.op("act", lambda e: e.copy(out=out, in_=in_), reads, writes)
            else:
                S.op("dve", lambda e: e.tensor_copy(out=out, in_=in_), reads, writes)

        def MSET(out, val, writes):
            S.op("dve", lambda e: e.memset(out, val), (), writes)

        def MM(out, lhsT, rhs, start, stop, reads, writes):
            S.op("pe", lambda e: e.matmul(out, lhsT=lhsT, rhs=rhs, start=start, stop=stop), reads, writes)

        def TR(out, in_, reads, writes):
            S.op("pe", lambda e: e.transpose(out=out, in_=in_, identity=ident), list(reads) + ["ident"], writes)

        def DMA(q, out, in_, reads, writes, key):
            S.op(q, lambda e: e.dma_start(out=out, in_=in_), reads, writes, dma_key=key)

        def mmgroup(out, pairs, reads, writes):
            pairs = list(pairs)

            def fn(e):
                ins = None
                n = len(pairs)
                for i, (l_, r_) in enumerate(pairs):
                    ins = e.matmul(out, lhsT=l_, rhs=r_, start=(i == 0), stop=(i == n - 1))
                return ins
            S.op("pe", fn, reads, writes)

        xs = A.alloc([128, KC, T], F32)
        hT = A.alloc([128, KC, T], BF16)
        wslots = [A.alloc([128, SLOT_ELEMS], BF16) for _ in range(NW)]
        ident = A.alloc([128, 128], BF16)
        ones_f = A.alloc([128, 128], F32)
        ones_b = A.alloc([128, 128], BF16)
        rg = A.alloc([128, 512], F32)
        RG = rg[:, 0:256]
        RO = rg[:, 256:512]
        etab = A.alloc([128, 8, 32], F32)
        gmask = A.alloc([128, 8, 32], F32)
        epsc = A.alloc([128, 4], F32)
        gcols = A.alloc([128, 64], F32)
        kmT_m = A.alloc([128, 8, 256], BF16)
        vm = A.alloc([128, 2, 1024], BF16)
        fz = A.alloc([128, 8], F32)

        def FENCE(old, new):
            old = list(old)
            new = list(new)
            S.op("dve", lambda e: e.memset(fz[:, 0:1], 0.0), old, old + new)

        def toks(name, n):
            return [(name, i) for i in range(n)]
        P5T = toks("sq", 3) + toks("rr", 3) + toks("aT", 2) + toks("sgf", 2)
        base = A.mark()
        RSZ = 16384
        nbytes_phase = SB_BYTES - base

        def region(r0, nb):
            assert r0 + nb <= nbytes_phase, (r0, nb, nbytes_phase)
            a = Arena(arena_t[:], base + r0 + nb)
            a.off = base + r0
            return a

        wstate = {"n": 0}

        def wload(src, a, b):
            i = wstate["n"] % NW
            wstate["n"] += 1
            assert a * b <= SLOT_ELEMS
            view = wslots[i][:, 0:a * b].rearrange("p (a b) -> p a b", a=a)
            DMA("pool", view, src, (), [("w", i), ("wh", 2 * i), ("wh", 2 * i + 1)], ("w", i))
            return view, ("w", i)

        def wload_h(src, a, b):
            hi = wstate.get("h", 0) % (2 * NW)
            wstate["h"] = wstate.get("h", 0) + 1
            assert a * b <= SLOT_ELEMS // 2
            o_ = (hi % 2) * (SLOT_ELEMS // 2)
            view = wslots[hi // 2][:, o_:o_ + a * b].rearrange("p (a b) -> p a b", a=a)
            DMA("pool", view, src, (), [("wh", hi), ("w", hi // 2)], ("wh", hi))
            return view, ("wh", hi)

        def wcols(w, c0, ncol, r0=0, nk=KC):
            return w[r0:r0 + nk * 128, c0:c0 + ncol].rearrange("(kc p) c -> p kc c", p=128)

        DMA("sp", ident, ident_in[:, :], (), ["ident"], "c0")
        DMA("sp", rg, rg_in[:, :], (), ["rg"], "c1")
        DMA("sp", etab.rearrange("p a b -> p (a b)"), etab_in[0:1, :].to_broadcast([128, 256]), (), ["etab"], "c2")
        DMA("sp", gmask.rearrange("p a b -> p (a b)"), gmask_in[0:1, :].to_broadcast([128, 256]), (), ["gmask"], "c3")
        MSET(ones_f, 1.0, ["ones_f"])
        MSET(ones_b, 1.0, ["ones_b"])
        MSET(epsc[:, 0:1], EPS, ["epsc"])
        MSET(epsc[:, 1:2], 128 * EPS, ["epsc"])
        xv = xT_in.rearrange("(kc p) t -> p kc t", p=128)
        for q4 in range(4):
            DMA("sp", xs[:, q4 * 4:(q4 + 1) * 4, :], xv[:, q4 * 4:(q4 + 1) * 4, :], (), ["xs"], ("x", q4))

        def rms_feature_major(src, ntok, gcol0, dst, src_tok, dst_tok, sq_bufs, rr_bufs, ps_ss, nkc=KC, dim=D):
            nhalf = max(1, ntok // 512)
            w = min(512, ntok)
            for hf in range(nhalf):
                sl = slice(hf * w, (hf + 1) * w)
                pss = ps_ss.next()
                for kc in range(nkc):
                    sq, sqt = sq_bufs.next()
                    ACT(sq[:, 0:w], src[:, kc, sl], AF.Square, [src_tok], [sqt])
                    MM(PS[pss][:, 0:w], ones_f, sq[:, 0:w], kc == 0, kc == nkc - 1, [sqt, "ones_f"], [("ps", pss)])
                rr, rrt = rr_bufs.next()
                ACT(rr[:, 0:w], PS[pss][:, 0:w], AF.Sqrt, [("ps", pss), "epsc"], [rrt], bias=epsc[:, 0:1], scale=1.0 / dim)
                RECIP(rr[:, 0:w], rr[:, 0:w], [rrt], [rrt])
                for kc in range(nkc):
                    STT(dst[:, kc, sl], src[:, kc, sl], gcols[:, gcol0 + kc:gcol0 + kc + 1], rr[:, 0:w], ALU.mult, ALU.mult,
                        [src_tok, rrt, "gcols"], [dst_tok])

        def mkbufs(ar, n, shape, dt, name):
            return Rot([(ar.alloc(shape, dt), (name, i)) for i in range(n)])

        for l in range(n_layers):
            W = L[l]
            w_in = W["w_in"]
            DMA("sp", gcols, W["gcols"][:, :], (), ["gcols"], "gc")

            FENCE(P5T, ["memT", "hmT"] + toks("sq", 2) + toks("rr", 2))
            a0 = region(0, RSZ)
            memT = a0.alloc([128, KC, 256], F32)
            a1 = region(RSZ, RSZ)
            hmT = a1.alloc([128, KC, 256], BF16)
            sq_bufs = mkbufs(a1, 2, [128, 512], F32, "sq")
            rr_bufs = mkbufs(a1, 2, [128, 512], F32, "rr")
            DMA("sp", memT, memT_in.rearrange("(kc p) t -> p kc t", p=128), (), ["memT"], "mem")
            rms_feature_major(memT, 256, 32, hmT, "memT", "hmT", sq_bufs, rr_bufs, Rot([6, 7]))
            psa = Rot([0, 1, 2, 3])
            for hm in range(4):
                wt, wtok = wload(wcols(W["w_mem_kv"], hm * 256, 256), KC, 256)
                pa = [psa.next(), psa.next()]
                for dc in range(2):
                    mmgroup(PS[pa[dc]][:, 0:256], [(wt[:, kc, dc * 128:(dc + 1) * 128], hmT[:, kc, :]) for kc in range(KC)],
                            [wtok, "hmT"], [("ps", pa[dc])])
                pss = 4 + (hm % 2)
                for dc in range(2):
                    sq, sqt = sq_bufs.next()
                    ACT(sq[:, 0:256], PS[pa[dc]][:, 0:256], AF.Square, [("ps", pa[dc])], [sqt])
                    MM(PS[pss][:, 0:256], ones_f, sq[:, 0:256], dc == 0, dc == 1, [sqt, "ones_f"], [("ps", pss)])
                rr, rrt = rr_bufs.next()
                ACT(rr[:, 0:256], PS[pss][:, 0:256], AF.Sqrt, [("ps", pss), "epsc"], [rrt], bias=epsc[:, 0:1], scale=1.0 / 256)
                RECIP(rr[:, 0:256], rr[:, 0:256], [rrt], [rrt])
                for dc in range(2):
                    STT(kmT_m[:, hm * 2 + dc, :], PS[pa[dc]][:, 0:256], gcols[:, 60 + dc:61 + dc], rr[:, 0:256], ALU.mult, ALU.mult,
                        [("ps", pa[dc]), rrt, "gcols"], ["kmT_m"])
            for vt in range(4):
                wt, wtok = wload(wcols(W["w_mem_kv"], 1024 + vt * 256, 256), KC, 256)
                for mc in range(2):
                    p = psa.next()
                    mmgroup(PS[p][:, 0:256], [(hmT[:, kc, mc * 128:(mc + 1) * 128], wt[:, kc, :]) for kc in range(KC)],
                            [wtok, "hmT"], [("ps", p)])
                    CPY("act", vm[:, mc, vt * 256:(vt + 1) * 256], PS[p][:, 0:256], [("ps", p)], ["vm"])

            if STOP_AFTER == 1:
                break
            FENCE(["memT", "hmT"] + toks("sq", 2) + toks("rr", 2), toks("sq", 3) + toks("rr", 3) + ["kms", "kmb", "qT", "kT", "Vl"])
            a0 = region(0, RSZ)
            sq_bufs = mkbufs(a0, 3, [128, 512], F32, "sq")
            rr_bufs = mkbufs(a0, 3, [128, 512], F32, "rr")
            kms = a0.alloc([128, 32], F32)
            kmb = a0.alloc([128, 32], BF16)
            qT = region(RSZ, RSZ).alloc([128, NH, T], BF16)
            kT = region(2 * RSZ, RSZ).alloc([128, NH, T], BF16)
            Vl = region(3 * RSZ, RSZ).alloc([128, NH, 8, 128], BF16)
            rms_feature_major(xs, T, 0, hT, "xs", "hT", sq_bufs, rr_bufs, Rot([6, 7]))
            psa = Rot([0, 1, 2, 3])
            psb_r = Rot([4, 5])

            def head_norm_proj(col0, dst, dst_tok, gcol, sqrt_scale, eps_col):
                pend = []

                def finish(item):
                    p, h, hf = item
                    sq, sqt = sq_bufs.next()
                    ACT(sq, PS[p], AF.Square, [("ps", p)], [sqt])
                    pb = psb_r.next()
                    MM(PS[pb], ones_f, sq, True, True, [sqt, "ones_f"], [("ps", pb)])
                    rr, rrt = rr_bufs.next()
                    ACT(rr, PS[pb], AF.Sqrt, [("ps", pb), "epsc"], [rrt], bias=epsc[:, eps_col:eps_col + 1], scale=sqrt_scale)
                    RECIP(rr, rr, [rrt], [rrt])
                    STT(dst[:, h, hf * 512:(hf + 1) * 512], PS[p], gcols[:, gcol:gcol + 1], rr, ALU.mult, ALU.mult,
                        [("ps", p), rrt, "gcols"], [dst_tok])

                for hp in range(4):
                    wt, wtok = wload(wcols(w_in, col0 + hp * 256, 256), KC, 256)
                    for hh in range(2):
                        h = hp * 2 + hh
                        for hf in range(2):
                            p = psa.next()
                            mmgroup(PS[p], [(wt[:, kc, hh * 128:(hh + 1) * 128], hT[:, kc, hf * 512:(hf + 1) * 512]) for kc in range(KC)],
                                    [wtok, "hT"], [("ps", p)])
                            pend.append((p, h, hf))
                            if len(pend) > 1:
                                finish(pend.pop(0))
                while pend:
                    finish(pend.pop(0))

            head_norm_proj(0, qT, "qT", 48, 1.0, 1)
            head_norm_proj(1024, kT, "kT", 49, 1.0 / 128, 0)
            for h in range(NH):
                RSUM(kms[:, h * 4:(h + 1) * 4], kT[:, h, :].rearrange("p (b t) -> p b t", b=4), ["kT"], ["kms"])
            TS(kmb, kms, 1.0 / 256, None, ALU.mult, None, ["kms"], ["kmb"])
            for vt in range(4):
                wt, wtok = wload(wcols(w_in, 2048 + vt * 256, 256), KC, 256)
                for lc in range(8):
                    p = psa.next()
                    mmgroup(PS[p][:, 0:256], [(hT[:, kc, lc * 128:(lc + 1) * 128], wt[:, kc, :]) for kc in range(KC)],
                            [wtok, "hT"], [("ps", p)])
                    CPY("act", Vl[:, vt * 2:vt * 2 + 2, lc, :], PS[p][:, 0:256].rearrange("p (a b) -> p a b", a=2), [("ps", p)], ["Vl"])
            DMA("sp", kvloc[l][:, 0:8192], kT.rearrange("p h t -> p (h t)"), ["kT"], [("kvloc", l)], "kv0")
            DMA("sp", kvloc[l][:, 8192:16384], Vl.rearrange("p h c d -> p (h c d)"), ["Vl"], [("kvloc", l)], "kv1")
            DMA("sp", kvloc[l][:, 16384:16416], kmb, ["kmb"], [("kvloc", l)], "kv2")
            kl, ka = kvloc[l], kvall[l]
            S.op("pool", lambda e, kl=kl, ka=ka: e.collective_compute("AllGather", ALU.bypass, replica_groups=[list(range(NCORE))],
                                                                      ins=[kl[:, :]], outs=[ka[:, :]]),
                 [("kvloc", l)], [("kvall", l)], dma_key=("cc", l), inc_amt=1)

            if STOP_AFTER == 2:
                break
            P2T = (["kmg", "kmT"] + toks("Kg", 2) + toks("Vg", 2) + toks("sc", 3) + toks("P", 4) + toks("PT", 3) + toks("gs", 2) + toks("mx", 2)
                   + toks("bias", 2) + toks("den", 2) + toks("ya", 2) + [("rs", rb_, par_, c_) for rb_ in range(2) for par_ in range(2) for c_ in range(40)])
            FENCE(toks("sq", 3) + toks("rr", 3) + ["kms", "kmb", "kT", "Vl"], ["yaT"] + P2T)
            yaT = region(0, RSZ).alloc([128, NH, T], BF16)
            a2 = region(2 * RSZ, nbytes_phase - 2 * RSZ)
            kmg = a2.alloc([128, 8, 32], BF16)
            kmT = a2.alloc([128, NH, 32], BF16)
            NKV = 2
            Kg = [a2.alloc([128, 8, 256], BF16) for _ in range(NKV)]
            Vg = [a2.alloc([128, 8, 256], BF16) for _ in range(NKV)]
            NSC = 3
            scb = [a2.alloc([128, 256], F32) for _ in range(NSC)]
            Pb = [a2.alloc([128, 256], BF16) for _ in range(4)]
            PTb = [a2.alloc([128, 256], BF16) for _ in range(3)]
            gsb = [a2.alloc([128, 32], F32) for _ in range(2)]
            mx8 = [a2.alloc([128, 8], F32) for _ in range(2)]
            biasb = [a2.alloc([128, 8, 32], F32) for _ in range(2)]
            rsb = [a2.alloc([128, 2, 40], F32) for _ in range(2)]
            den = [a2.alloc([128, 2], F32) for _ in range(2)]
            yab = [a2.alloc([128, 128], BF16) for _ in range(2)]
            kvall_v = kvall[l].rearrange("(c p) f -> p c f", p=128)
            DMA("sp", kmg, kvall_v[:, :, 16384:16416], [("kvall", l)], ["kmg"], "kmg")
            for h in range(NH):
                CPY("dve", kmT[:, h, :].rearrange("p (b c) -> p b c", b=4), kmg[:, :, h * 4:(h + 1) * 4].rearrange("p c b -> p b c"), ["kmg"], ["kmT"])
            kv_rot = Rot(list(range(NKV)))
            sc_rot = Rot(list(range(NSC)))
            p_rot = Rot(list(range(4)))
            pt_rot = Rot(list(range(3)))
            sps_rot = Rot([2, 3, 4])
            ptps_rot = Rot([5, 6])

            def attn_pair(h, lbq, slope, hb, rb):
                rs = rsb[rb]
                rs_all = [("rs", rb, par_, c_) for par_ in range(2) for c_ in range(40)]
                MSET(rs, 0.0, rs_all)
                units = []
                loads = []
                for lbk in range(lbq + 1):
                    kb = kv_rot.next()
                    loads.append((kb, lbk, False))
                    nb = 8 if lbk < lbq else 7
                    for c in range(nb):
                        for par in range(2):
                            units.append((par, "past", kb, c, lbk * 8 + c))
                kb = kv_rot.next()
                loads.append((kb, lbq, True))
                units.append((0, "own", kb, 0, -1))
                units.append((1, "own", kb, 0, -1))
                load_pos = {}
                ui = 0
                for kb, lbk, own in loads:
                    load_pos[ui] = (kb, lbk, own)
                    ui += 2 if own else 2 * (8 if lbk < lbq else 7)

                def do_load(kb, lbk, own):
                    co = h * 1024 + lbk * 256
                    if own:
                        DMA("sp", Kg[kb][:, 0, :], kvloc[l][:, co:co + 256], [("kvloc", l)], [("Kg", kb)], ("K", kb))
                        DMA("sp", Vg[kb][:, 0, :], kvloc[l][:, 8192 + co:8192 + co + 256], [("kvloc", l)], [("Vg", kb)], ("V", kb))
                    else:
                        DMA("sp", Kg[kb], kvall_v[:, :, co:co + 256], [("kvall", l)], [("Kg", kb)], ("K", kb))
                        DMA("sp", Vg[kb], kvall_v[:, :, 8192 + co:8192 + co + 256], [("kvall", l)], [("Vg", kb)], ("V", kb))

                nun = [0, 0]
                first = [True, True]
                last_idx = [max(i for i, u in enumerate(units) if u[0] == par) for par in range(2)]
                stu = []

                def stage_a(i):
                    if i in load_pos:
                        do_load(*load_pos[i])
                    par, kind, kb, blk, n = units[i]
                    lc = lbq * 2 + par
                    ncol = 128 if (kind == "own" and par == 0) else 256
                    sp_ = sps_rot.next()
                    spv = PS[sp_][:, 0:ncol]
                    stok = ("ps", sp_)
                    MM(spv, qT[:, h, lc * 128:(lc + 1) * 128], Kg[kb][:, blk, 0:ncol], True, True, ["qT", ("Kg", kb)], [stok])
                    si = sc_rot.next()
                    sc = scb[si][:, 0:ncol]
                    if kind == "past":
                        rmat = RG
                    else:
                        rmat = RO[:, 128:256] if par == 0 else RO
                    STT(sc, rmat, slope, spv, ALU.mult, ALU.add, [stok, "rg"], [("sc", si)])
                    pi = p_rot.next()
                    pv = Pb[pi][:, 0:ncol]
                    col = nun[par]
                    nun[par] += 1
                    if kind == "past":
                        ACT(pv, sc, AF.Exp, [("sc", si), ("bias", hb)], [("P", pi), ("rs", rb, par, col)],
                            bias=biasb[hb][:, lc, n:n + 1], scale=1.0, accum_out=rs[:, par, col:col + 1])
                    else:
                        ACT(pv, sc, AF.Exp, [("sc", si)], [("P", pi), ("rs", rb, par, col)], accum_out=rs[:, par, col:col + 1])
                    stu.append(dict(pi=pi, ncol=ncol, par=par, kb=kb, blk=blk, i=i))

                def stage_b(j):
                    u = stu[j]
                    tp_ = ptps_rot.next()
                    nk = u["ncol"] // 128
                    ttok = ("ps", tp_)
                    for k2 in range(nk):
                        TR(PSB[tp_][:, k2 * 128:(k2 + 1) * 128], Pb[u["pi"]][:, k2 * 128:(k2 + 1) * 128], [("P", u["pi"])], [ttok])
                    ti = pt_rot.next()
                    u["ti"] = ti
                    CPY("dve", PTb[ti][:, 0:u["ncol"]], PSB[tp_][:, 0:u["ncol"]], [ttok], [("PT", ti)])

                def stage_c(j):
                    u = stu[j]
                    par = u["par"]
                    nk = u["ncol"] // 128
                    for k2 in range(nk):
                        st_ = first[par] and k2 == 0
                        sp2 = (u["i"] == last_idx[par]) and k2 == nk - 1
                        MM(PS[par][:, 0:128], PTb[u["ti"]][:, k2 * 128:(k2 + 1) * 128], Vg[u["kb"]][:, u["blk"], k2 * 128:(k2 + 1) * 128], st_, sp2,
                           [("PT", u["ti"]), ("Vg", u["kb"])], [("ps", par)])
                        first[par] = False

                nu = len(units)
                for i in range(nu + 3):
                    if i < nu:
                        stage_a(i)
                    if 0 <= i - 2 < nu:
                        stage_b(i - 2)
                    if 0 <= i - 3 < nu:
                        stage_c(i - 3)
                dn = den[rb]
                RSUM(dn, rs, rs_all, [("den", rb)])
                RECIP(dn, dn, [("den", rb)], [("den", rb)])
                for par in range(2):
                    lc = lbq * 2 + par
                    yi = lc % 2
                    ACT(yab[yi], PS[par][:, 0:128], AF.Copy, [("ps", par), ("den", rb)], [("ya", yi)], scale=dn[:, par:par + 1])
                    TR(PSB[7][:, 0:128], yab[yi], [("ya", yi)], [("ps", 7)])
                    CPY("act", yaT[:, h, lc * 128:(lc + 1) * 128], PSB[7][:, 0:128], [("ps", 7)], ["yaT"])

            for h in range(NH):
                slope = 2.0 ** (-(h + 1))
                hb = h % 2
                for lc in range(8):
                    MM(PS[7][:, lc * 32:(lc + 1) * 32], qT[:, h, lc * 128:(lc + 1) * 128], kmT[:, h, :], True, True, ["qT", "kmT"], [("ps", 7)])
                for lc in range(8):
                    g, gtk = gsb[lc % 2], ("gs", lc % 2)
                    m8, m8t = mx8[lc % 2], ("mx", lc % 2)
                    TT(g, PS[7][:, lc * 32:(lc + 1) * 32], gmask[:, lc, :], ALU.add, [("ps", 7), "gmask"], [gtk])
                    S.op("dve", lambda e, g=g, m8=m8: e.max(out=m8, in_=g), [gtk], [m8t])
                    TS(g, g, m8[:, 2:3], -30000.0, ALU.is_lt, ALU.mult, [gtk, m8t], [gtk])
                    STT(biasb[hb][:, lc, :], etab[:, lc, :], slope, g, ALU.mult, ALU.add, [gtk, "etab"], [("bias", hb)])
                for lbq in range(4):
                    attn_pair(h, lbq, slope, hb, (h * 4 + lbq) % 2)

            if STOP_AFTER == 3:
                break
            SGT = ["gsg", "bsb", "wsb", "trl", "vjk", "ssv", "rsv"] + toks("vgf", 2)
            FENCE(["qT"] + P2T, SGT + ["ybT", "vtm"])
            ybT = region(2 * RSZ, RSZ).alloc([128, 8, T], BF16)
            vtm = region(3 * RSZ, RSZ).alloc([128, 8, 1024], BF16)
            a1 = region(RSZ, RSZ)
            gsg = a1.alloc([128, 1024], F32)
            bsb = a1.alloc([128, 8, 128], F32)
            wsb = a1.alloc([128, 8, 128], BF16)
            trl = a1.alloc([128, 128], BF16)
            un = a1.alloc([128, 2048], BF16)
            vgf = [un[:, i * 512:(i + 1) * 512].bitcast(F32) for i in range(2)]
            uT = [un[:, i * 1024:(i + 1) * 1024] for i in range(2)]
            tj = a1.alloc([128, 256], F32)
            vjk = tj
            tmpb = [tj[:, i * 128:(i + 1) * 128] for i in range(2)]
            ssv = a1.alloc([128, 8, 4], F32)
            rsv = a1.alloc([128, 8], F32)
            DMA("sp", gsg, W["g_sgu"][0:1, :].to_broadcast([128, 1024]), (), ["gsg"], "s0")
            DMA("sp", bsb.rearrange("p a b -> p (a b)"), W["b_sgu"][0:1, :].to_broadcast([128, 1024]), (), ["bsb"], "s1")
            DMA("pool", wsb.rearrange("p a b -> p (a b)"), W["wsT"][:, :], (), ["wsb"], "s2")
            DMA("sp", trl, tril_in[:, :], (), ["trl"], "s3")
            for g in range(8):
                TT(wsb[:, g, :], wsb[:, g, :], trl, ALU.mult, ["wsb", "trl"], ["wsb"])
            MSET(ssv, 0.0, ["ssv"])
            psa = Rot([0, 1, 2, 3])
            for vt in range(4):
                wt, wtok = wload(wcols(w_in, 4096 + vt * 256, 256), KC, 256)
                for lc in range(8):
                    p = psa.next()
                    mmgroup(PS[p][:, 0:256], [(hT[:, kc, lc * 128:(lc + 1) * 128], wt[:, kc, :]) for kc in range(KC)],
                            [wtok, "hT"], [("ps", p)])
                    vb_ = (vt * 8 + lc) % 2
                    ACT(vgf[vb_], PS[p][:, 0:256], AF.Gelu, [("ps", p)], [("vgf", vb_)])
                    ACT(vjk, vgf[vb_], AF.Square, [("vgf", vb_), "ssv"], ["vjk", "ssv"], accum_out=ssv[:, lc, vt:vt + 1])
                    CPY("dve", vtm[:, lc, vt * 256:(vt + 1) * 256], vgf[vb_], [("vgf", vb_)], ["vtm"])
            RSUM(rsv, ssv, ["ssv"], ["rsv"])
            ACT(rsv, rsv, AF.Sqrt, ["rsv", "epsc"], ["rsv"], bias=epsc[:, 0:1], scale=1.0 / 1024)
            RECIP(rsv, rsv, ["rsv"], ["rsv"])
            for lc in range(8):
                STT(vtm[:, lc, :], vtm[:, lc, :], rsv[:, lc:lc + 1], gsg, ALU.mult, ALU.mult, ["vtm", "rsv", "gsg"], ["vtm"])
            FENCE(toks("vgf", 2) + ["vjk"], toks("uT", 2) + toks("tmpb", 2))
            for gp in range(4):
                wt, wtok = wload(wcols(w_in, 3072 + gp * 256, 256), KC, 256)
                for gg in range(2):
                    g = gp * 2 + gg
                    ub = g % 2
                    for hf in range(2):
                        p = psa.next()
                        mmgroup(PS[p], [(wt[:, kc, gg * 128:(gg + 1) * 128], hT[:, kc, hf * 512:(hf + 1) * 512]) for kc in range(KC)],
                                [wtok, "hT"], [("ps", p)])
                        ACT(uT[ub][:, hf * 512:(hf + 1) * 512], PS[p], AF.Gelu, [("ps", p)], [("uT", ub)])
                    for lq in range(2):
                        p = 4 + (g * 2 + lq) % 2
                        for l4 in range(4):
                            lc = lq * 4 + l4
                            MM(PS[p][:, l4 * 128:(l4 + 1) * 128], vtm[:, lc, g * 128:(g + 1) * 128], wsb[:, g, :], True, True, ["vtm", "wsb"], [("ps", p)])
                        for l4 in range(4):
                            lc = lq * 4 + l4
                            tb = l4 % 2
                            TT(tmpb[tb], PS[p][:, l4 * 128:(l4 + 1) * 128], bsb[:, g, :], ALU.add, [("ps", p), "bsb"], [("tmpb", tb)])
                            TT(ybT[:, g, lc * 128:(lc + 1) * 128], tmpb[tb], uT[ub][:, lc * 128:(lc + 1) * 128], ALU.mult, [("tmpb", tb), ("uT", ub)], ["ybT"])

            if STOP_AFTER == 4:
                break
            M3T = toks("sq", 2) + toks("rr", 2) + toks("qmT", 2) + toks("PTm", 2)
            FENCE(SGT + toks("uT", 2) + toks("tmpb", 2) + ["vtm"], ["ymT"] + M3T)
            ymT = region(RSZ, RSZ).alloc([128, 8, T], BF16)
            a3 = region(3 * RSZ, nbytes_phase - 3 * RSZ)
            sq_bufs = mkbufs(a3, 2, [128, 512], F32, "sq")
            rr_bufs = mkbufs(a3, 2, [128, 512], F32, "rr")
            qmT = [a3.alloc([128, 2, 512], BF16) for _ in range(2)]
            PTm = [a3.alloc([128, 2, 512], BF16) for _ in range(2)]
            psa = Rot([0, 1, 2, 3])
            it = 0
            for hm in range(4):
                wt, wtok = wload(wcols(w_in, 5120 + hm * 256, 256), KC, 256)
                for hf in range(2):
                    qb = it % 2
                    it += 1
                    pa = [psa.next(), psa.next()]
                    for dc in range(2):
                        mmgroup(PS[pa[dc]], [(wt[:, kc, dc * 128:(dc + 1) * 128], hT[:, kc, hf * 512:(hf + 1) * 512]) for kc in range(KC)],
                                [wtok, "hT"], [("ps", pa[dc])])
                    pss = 4
                    for dc in range(2):
                        sq, sqt = sq_bufs.next()
                        ACT(sq, PS[pa[dc]], AF.Square, [("ps", pa[dc])], [sqt])
                        MM(PS[pss], ones_f, sq, dc == 0, dc == 1, [sqt, "ones_f"], [("ps", pss)])
                    rr, rrt = rr_bufs.next()
                    ACT(rr, PS[pss], AF.Sqrt, [("ps", pss), "epsc"], [rrt], bias=epsc[:, 0:1], scale=1.0 / 256)
                    RECIP(rr, rr, [rrt], [rrt])
                    for dc in range(2):
                        STT(qmT[qb][:, dc, :], PS[pa[dc]], gcols[:, 50 + dc:51 + dc], rr, ALU.mult, ALU.mult,
                            [("ps", pa[dc]), rrt, "gcols"], [("qmT", qb)])
                    for mc in range(2):
                        p = psa.next()
                        mmgroup(PS[p], [(kmT_m[:, hm * 2 + dc, mc * 128:(mc + 1) * 128], qmT[qb][:, dc, :]) for dc in range(2)],
                                ["kmT_m", ("qmT", qb)], [("ps", p)])
                        ACT(PTm[qb][:, mc, :], PS[p], AF.Exp, [("ps", p)], [("PTm", qb)], scale=1.0 / 16)
                    mmgroup(PS[5], [(ones_b, PTm[qb][:, mc, :]) for mc in range(2)], ["ones_b", ("PTm", qb)], [("ps", 5)])
                    rr2, rrt2 = rr_bufs.next()
                    RECIP(rr2, PS[5], [("ps", 5)], [rrt2])
                    for dc in range(2):
                        p = 6 + dc
                        mmgroup(PS[p], [(vm[:, mc, hm * 256 + dc * 128:hm * 256 + (dc + 1) * 128], PTm[qb][:, mc, :]) for mc in range(2)],
                                ["vm", ("PTm", qb)], [("ps", p)])
                        TT(ymT[:, hm * 2 + dc, hf * 512:(hf + 1) * 512], PS[p], rr2, ALU.mult, [("ps", p), rrt2], ["ymT"])

            if STOP_AFTER == 5:
                for kc in range(8):
                    CPY("dve", xs[:, kc, :], yaT[:, kc, :], ["yaT", "xs"], ["xs"])
                    CPY("dve", xs[:, 8 + kc, :], ybT[:, kc, :], ["ybT", "xs"], ["xs"])
                break
            P4T = toks("mg", 2) + toks("sg", 3) + toks("tf", 3)
            FENCE(M3T, P4T)
            a4 = region(3 * RSZ, nbytes_phase - 3 * RSZ)
            mg = [a4.alloc([128, 2, T], BF16) for _ in range(2)]
            sgb = [a4.alloc([128, 512], BF16) for _ in range(3)]
            tf = [a4.alloc([128, 512], F32) for _ in range(3)]
            yTs = [yaT, ybT, ymT]
            ytok = ["yaT", "ybT", "ymT"]
            for G in range(8):
                mb = G % 2
                for jj in range(2):
                    j = G * 2 + jj
                    gw = [wload_h(wcols(w_in, 6144 + b * 2048 + j * 128, 128), KC, 128) for b in range(3)]
                    bw = [wload_h(W["w_branch"][b, :, j * 128:(j + 1) * 128].rearrange("(kc p) c -> p kc c", p=128), 8, 128) for b in range(3)]
                    for hf in range(2):
                        tsl = slice(hf * 512, (hf + 1) * 512)
                        for b in range(3):
                            wt, wtok = gw[b]
                            mmgroup(PS[b], [(wt[:, kc, :], hT[:, kc, tsl]) for kc in range(KC)], [wtok, "hT"], [("ps", b)])
                            ACT(sgb[b], PS[b], AF.Sigmoid, [("ps", b)], [("sg", b)])
                        for b in range(3):
                            wt, wtok = bw[b]
                            mmgroup(PS[3 + b], [(wt[:, kc, :], yTs[b][:, kc, tsl]) for kc in range(8)], [wtok, ytok[b]], [("ps", 3 + b)])
                            TT(tf[b], PS[3 + b], sgb[b], ALU.mult, [("ps", 3 + b), ("sg", b)], [("tf", b)])
                        TT(tf[0], tf[0], tf[1], ALU.add, [("tf", 0), ("tf", 1)], [("tf", 0)])
                        TT(mg[mb][:, jj, tsl], tf[0], tf[2], ALU.add, [("tf", 0), ("tf", 2)], [("mg", mb)])
                for jq in range(4):
                    wt, wtok = wload(W["w_out"][G * 256:(G + 1) * 256, jq * 512:(jq + 1) * 512].rearrange("(kc p) c -> p kc c", p=128), 2, 512)
                    for jt in range(4):
                        jd = jq * 4 + jt
                        for hf in range(2):
                            tsl = slice(hf * 512, (hf + 1) * 512)
                            p = 6 + (jt * 2 + hf) % 2
                            mmgroup(PS[p], [(wt[:, kc, jt * 128:(jt + 1) * 128], mg[mb][:, kc, tsl]) for kc in range(2)], [wtok, ("mg", mb)], [("ps", p)])
                            TT(xs[:, jd, tsl], xs[:, jd, tsl], PS[p], ALU.add, [("ps", p), "xs"], ["xs"])

            if STOP_AFTER == 6:
                break
            a5 = region(0, nbytes_phase)
            sq_bufs = mkbufs(a5, 3, [128, 512], F32, "sq")
            rr_bufs = mkbufs(a5, 3, [128, 512], F32, "rr")
            aT = [a5.alloc([128, 8, T], BF16) for _ in range(2)]
            sgf = [a5.alloc([128, 512], BF16) for _ in range(2)]
            FENCE(["yaT", "ybT", "ymT"] + P4T, P5T)
            rms_feature_major(xs, T, 16, hT, "xs", "hT", sq_bufs, rr_bufs, Rot([6, 7]))
            psg = Rot([0, 1, 2, 3, 4, 5])
            n_it = 0
            for F in range(6):
                ab = F % 2
                nch = 8 if F < 5 else 4
                for pr in range(nch // 2):
                    c0 = F * 1024 + pr * 256
                    gt_w, gtok = wload(wcols(W["w_gate_up"], c0, 256), KC, 256)
                    up_w, utok = wload(wcols(W["w_gate_up"], DFF + c0, 256), KC, 256)
                    for jj in range(2):
                        cc = pr * 2 + jj
                        for hf in range(2):
                            tsl = slice(hf * 512, (hf + 1) * 512)
                            pg = psg.next()
                            pu = psg.next()
                            mmgroup(PS[pg], [(gt_w[:, kc, jj * 128:(jj + 1) * 128], hT[:, kc, tsl]) for kc in range(KC)], [gtok, "hT"], [("ps", pg)])
                            mmgroup(PS[pu], [(up_w[:, kc, jj * 128:(jj + 1) * 128], hT[:, kc, tsl]) for kc in range(KC)], [utok, "hT"], [("ps", pu)])
                            sb_ = n_it % 2
                            n_it += 1
                            ACT(sgf[sb_], PS[pg], AF.Silu, [("ps", pg)], [("sgf", sb_)])
                            TT(aT[ab][:, cc, tsl], PS[pu], sgf[sb_], ALU.mult, [("ps", pu), ("sgf", sb_)], [("aT", ab)])
                for jq in range(4):
                    wt, wtok = wload(W["w_down"][F * 1024:F * 1024 + nch * 128, jq * 512:(jq + 1) * 512].rearrange("(kc p) c -> p kc c", p=128), nch, 512)
                    for jt in range(4):
                        jd = jq * 4 + jt
                        for hf in range(2):
                            tsl = slice(hf * 512, (hf + 1) * 512)
                            p = 6 + (jt * 2 + hf) % 2
                            mmgroup(PS[p], [(wt[:, kc, jt * 128:(jt + 1) * 128], aT[ab][:, kc, tsl]) for kc in range(nch)], [wtok, ("aT", ab)], [("ps", p)])
                            TT(xs[:, jd, tsl], xs[:, jd, tsl], PS[p], ALU.add, [("ps", p), "xs"], ["xs"])

        ov = outT.rearrange("(kc p) t -> p kc t", p=128)
        for q4 in range(4):
            DMA("sp", ov[:, q4 * 4:(q4 + 1) * 4, :], xs[:, q4 * 4:(q4 + 1) * 4, :], ["xs"], (), ("o", q4))
        S.emit(st)
    return nc


def _const_tables():
    i = np.arange(128, dtype=np.float32)[:, None]
    j = np.arange(256, dtype=np.float32)[None, :]
    RGm = (j - i).astype(np.float32)
    dist = 128.0 + i - j
    ROm = np.where(dist >= 0, -dist, -1e7).astype(np.float32)
    rg = np.concatenate([RGm, ROm], axis=1)
    ident = np.eye(128, dtype=np.float32).astype(ml_dtypes.bfloat16)
    s = np.arange(128)[:, None]
    t = np.arange(128)[None, :]
    trilT = (s <= t).astype(np.float32).astype(ml_dtypes.bfloat16)
    return rg, ident, trilT


def _core_tables(c):
    et = np.full((8, 32), -1e7, np.float32)
    gm = np.full((8, 32), -1e30, np.float32)
    for lc in range(8):
        own = 8 * (lc // 2) + c
        gc = 2 * own + (lc % 2)
        for n in range(own):
            et[lc, n] = -(gc * 128 - n * 256)
            gm[lc, n] = 0.0
    return et.reshape(1, 256), gm.reshape(1, 256)


def _layer_inputs(l, s, g_mix, w_in, gq_a, gk_a, g_sgu, w_sgu, b_sgu, gq_m, gk_m, g_mem, w_mem_kv, w_branch, w_out, g_ffn, w_gate_up, w_down):
    gc = np.zeros((128, 64), np.float32)
    gc[:, 0:16] = g_mix[l].reshape(16, 128).T
    gc[:, 16:32] = g_ffn[l].reshape(16, 128).T
    gc[:, 32:48] = g_mem[l].reshape(16, 128).T
    gc[:, 48] = gq_a[l]
    gc[:, 49] = gk_a[l]
    gc[:, 50:52] = gq_m[l].reshape(2, 128).T
    gc[:, 60:62] = gk_m[l].reshape(2, 128).T
    wsT = np.ascontiguousarray(np.transpose(w_sgu[l], (2, 0, 1))).reshape(128, 8 * 128)
    return {
        "w_in" + s: w_in[l], "w_mem_kv" + s: w_mem_kv[l], "w_branch" + s: w_branch[l], "w_out" + s: w_out[l],
        "w_gate_up" + s: w_gate_up[l], "w_down" + s: w_down[l], "gcols" + s: gc,
        "g_sgu" + s: np.ascontiguousarray(g_sgu[l].reshape(1, 1024)), "wsT" + s: wsT,
        "b_sgu" + s: np.ascontiguousarray(b_sgu[l].reshape(1, 1024)),
    }


_PROGRAMS = {}
N_FUSED_LAYERS = 2


def _tokens_of_core(c):
    return np.concatenate([np.arange((8 * lb + c) * 256, (8 * lb + c + 1) * 256) for lb in range(4)])


def kernel(x, mem, g_mix, w_in, gq_a, gk_a, g_sgu, w_sgu, b_sgu, gq_m, gk_m, g_mem,
           w_mem_kv, w_branch, w_out, g_ffn, w_gate_up, w_down):
    args = [np.asarray(a, dtype=np.float32) for a in (g_mix, w_in, gq_a, gk_a, g_sgu, w_sgu, b_sgu, gq_m, gk_m, g_mem,
                                                       w_mem_kv, w_branch, w_out, g_ffn, w_gate_up, w_down)]
    x = np.asarray(x, dtype=np.float32)
    mem = np.asarray(mem, dtype=np.float32)
    depth = args[0].shape[0]
    nl = N_FUSED_LAYERS
    if nl not in _PROGRAMS:
        _PROGRAMS[nl] = build_program(nl)
    nc = _PROGRAMS[nl]
    rg, ident, trilT = _const_tables()
    memT = np.ascontiguousarray(mem[0].T)
    toks = [_tokens_of_core(c) for c in range(NCORE)]
    xT = [np.ascontiguousarray(x[0][toks[c]].T) for c in range(NCORE)]
    tabs = [_core_tables(c) for c in range(NCORE)]
    for l0 in range(0, depth, nl):
        lay = {}
        for li in range(nl):
            lay.update(_layer_inputs(l0 + li, "_%d" % li, *args))
        in_maps = []
        for c in range(NCORE):
            m = dict(lay)
            m.update({"xT": xT[c], "memT": memT, "ident": ident, "rg": rg, "etab": tabs[c][0], "gmask": tabs[c][1], "trilT": trilT})
            in_maps.append(m)
        res = run_bass_kernel_spmd(nc, in_maps, core_ids=list(range(NCORE)))
        xT = [np.ascontiguousarray(np.asarray(res.results[c]["outT"], dtype=np.float32)) for c in range(NCORE)]
    out = np.empty((1, x.shape[1], D), np.float32)
    for c in range(NCORE):
        out[0][toks[c]] = xT[c].T
    return out
```
